# Optimizing a Trainium2 kernel written in Bass

```python
import math
import jax
import jax.numpy as jnp
from jax import lax
import numpy as np


D_MODEL = 2048
BATCH = 2
SEQ = 8192
DEPTH = 4

GRID_W = 64
CTX_LEN = 256
D_MIX = D_MODEL
D_SSD = D_MIX // 2
SSD_HEAD_DIM = 64
SSD_HEADS = D_SSD // SSD_HEAD_DIM
SSD_GROUPS = 2
SSD_STATE = 128
SSD_XBC = D_SSD + 2 * SSD_GROUPS * SSD_STATE
D_RET = D_MIX // 4
RET_HEAD_DIM = 128
RET_HEADS = D_RET // RET_HEAD_DIM
D_GDN = D_MIX - D_SSD - D_RET
GDN_HEAD_DIM = 128
GDN_HEADS = D_GDN // GDN_HEAD_DIM
CONV_W = 5
CHUNK = 64
ROPE_BASE = 10000.0
SSD_COLS = D_SSD + SSD_XBC + 2 * SSD_HEADS
RET_COLS = 4 * D_RET
GDN_COLS = 4 * D_GDN + 4 * GDN_HEADS
N_IN_PROJ = SSD_COLS + RET_COLS + GDN_COLS
D_FF = 5632
N_EXPERTS = 8
TOP_K = 2
N_DENSE = (DEPTH + 1) // 2
N_MOE = DEPTH // 2
DEEPNORM_ALPHA = (2 * DEPTH) ** 0.25
DEEPNORM_BETA = (8 * DEPTH) ** -0.25
EPS = 1e-6

kernel_name = 'hybrid_ssd_retnet_gdn_moe_dit'


def layer_norm(x):
    xf = x.astype(jnp.float32)
    mu = jnp.mean(xf, axis=-1, keepdims=True)
    var = jnp.mean(jnp.square(xf - mu), axis=-1, keepdims=True)
    return ((xf - mu) * lax.rsqrt(var + EPS)).astype(x.dtype)


def rms_norm(x):
    xf = x.astype(jnp.float32)
    return (xf * lax.rsqrt(jnp.mean(jnp.square(xf), axis=-1, keepdims=True) + EPS)).astype(x.dtype)


def l2_norm(x):
    xf = x.astype(jnp.float32)
    return (xf * lax.rsqrt(jnp.sum(jnp.square(xf), axis=-1, keepdims=True) + EPS)).astype(x.dtype)


def modulate(x, shift, scale):
    return layer_norm(x) * (1.0 + scale) + shift


def post_norm(x, sub, g, b):
    return layer_norm(DEEPNORM_ALPHA * x + sub) * g + b


def depthwise_conv(x, w):
    return lax.conv_general_dilated(
        x, w[:, None, :].astype(x.dtype), window_strides=(1,),
        padding=[(CONV_W // 2, CONV_W // 2)],
        dimension_numbers=('NWC', 'WIO', 'NWC'), feature_group_count=x.shape[-1])


def rope_tables(rows, head_dim):
    n_freq = head_dim // 4
    inv_freq = ROPE_BASE ** (-jnp.arange(n_freq, dtype=jnp.float32) / n_freq)
    row = jnp.repeat(jnp.arange(rows, dtype=jnp.float32), GRID_W)
    col = jnp.tile(jnp.arange(GRID_W, dtype=jnp.float32), rows)
    ang = jnp.concatenate([row[:, None] * inv_freq, col[:, None] * inv_freq], axis=-1)
    return jnp.cos(ang)[None, :, None, :], jnp.sin(ang)[None, :, None, :]


def apply_rope(x, cos, sin):
    x1, x2 = jnp.split(x, 2, axis=-1)
    return jnp.concatenate([x1 * cos - x2 * sin, x2 * cos + x1 * sin], axis=-1)


def to_chunks(a):
    b_, L = a.shape[0], a.shape[1]
    a = a.reshape((b_, L // CHUNK, CHUNK) + a.shape[2:])
    return jnp.moveaxis(jnp.moveaxis(a, 1, 0), 2, 3)


def from_chunks(y):
    nc, b_, h_, l_, p_ = y.shape
    return jnp.moveaxis(jnp.moveaxis(y, 3, 2), 0, 1).reshape(b_, nc * l_, h_, p_)


def chunk_masks():
    tri = jnp.tril(jnp.ones((CHUNK, CHUNK), dtype=bool))
    return tri, jnp.tril(tri, -1)


def chunked_linear_scan(s0, q, k, v, log_a):
    b_, _, h_, n_ = q.shape
    p_ = v.shape[-1]
    q, k, v = (to_chunks(t.astype(jnp.float32)) for t in (q, k, v))
    g = jnp.cumsum(to_chunks(log_a.astype(jnp.float32)), axis=-1)
    tri, _ = chunk_masks()
    decay = jnp.exp(jnp.where(tri, g[..., :, None] - g[..., None, :], -jnp.inf))
    y_intra = jnp.einsum('cbhls,cbhsp->cbhlp', jnp.einsum('cbhln,cbhsn->cbhls', q, k) * decay, v)
    g_last = g[..., -1]
    chunk_states = jnp.einsum('cbhln,cbhlp->cbhnp', k * jnp.exp(g_last[..., None] - g)[..., None], v)
    if s0 is None:
        s0 = jnp.zeros((b_, h_, n_, p_), jnp.float32)

    def step(s, inp):
        st, d = inp
        return s * d[..., None, None] + st, s

    s_fin, s_in = lax.scan(step, s0, (chunk_states, jnp.exp(g_last)))
    y = y_intra + jnp.einsum('cbhln,cbhnp->cbhlp', q * jnp.exp(g)[..., None], s_in)
    return from_chunks(y), s_fin


def chunked_delta_scan(s0, q, k, v, beta, log_a):
    b_, _, h_, n_ = q.shape
    p_ = v.shape[-1]
    q, k, v = (to_chunks(t.astype(jnp.float32)) for t in (q, k, v))
    beta = to_chunks(beta.astype(jnp.float32))
    g = jnp.cumsum(to_chunks(log_a.astype(jnp.float32)), axis=-1)
    tri, strict = chunk_masks()
    incl = jnp.exp(jnp.where(tri, g[..., :, None] - g[..., None, :], -jnp.inf))
    kb = k * beta[..., None]
    t_mat = jnp.eye(CHUNK, dtype=jnp.float32) + jnp.where(strict, jnp.einsum('cbhln,cbhsn->cbhls', kb, k) * incl, 0.0)
    rhs = jnp.concatenate([v * beta[..., None], kb * jnp.exp(g)[..., None]], axis=-1)
    sol = lax.linalg.triangular_solve(t_mat, rhs, left_side=True, lower=True, unit_diagonal=True)
    u, w = sol[..., :p_], sol[..., p_:]
    attn = jnp.einsum('cbhln,cbhsn->cbhls', q, k) * incl
    g_last = g[..., -1]
    k_dec = k * jnp.exp(g_last[..., None] - g)[..., None]
    q_dec = q * jnp.exp(g)[..., None]
    if s0 is None:
        s0 = jnp.zeros((b_, h_, n_, p_), jnp.float32)

    def step(s, inp):
        q_c, k_c, u_c, w_c, a_c, d_c = inp
        v_new = u_c - jnp.einsum('bhln,bhnp->bhlp', w_c, s)
        o = jnp.einsum('bhln,bhnp->bhlp', q_c, s) + jnp.einsum('bhls,bhsp->bhlp', a_c, v_new)
        s = s * d_c[..., None, None] + jnp.einsum('bhln,bhlp->bhnp', k_c, v_new)
        return s, o

    s_fin, o = lax.scan(step, s0, (q_dec, k_dec, u, w, attn, jnp.exp(g_last)))
    return from_chunks(o), s_fin


def bidir_two_stream(scan, dirs_c, dirs_l):
    flip = lambda t: tuple(jnp.flip(a, axis=1) for a in t)
    y_cf, s_cf = scan(None, *dirs_c[0])
    y_cb, s_cb = scan(None, *flip(dirs_c[1]))
    y_lf, _ = scan(s_cf, *dirs_l[0])
    y_lb, _ = scan(s_cb, *flip(dirs_l[1]))
    return y_cf + jnp.flip(y_cb, axis=1), y_lf + jnp.flip(y_lb, axis=1)


def ssd_mixer(p_c, p_l, conv_w, conv_b, a_log, dt_bias, d_skip, norm_w):
    def prep(p):
        b_, L = p.shape[0], p.shape[1]
        z, xbc, dt = jnp.split(p, [D_SSD, D_SSD + SSD_XBC], axis=-1)
        xbc = jax.nn.silu(depthwise_conv(xbc, conv_w) + conv_b)
        xs, bm, cm = jnp.split(xbc, [D_SSD, D_SSD + SSD_GROUPS * SSD_STATE], axis=-1)
        xs = xs.reshape(b_, L, SSD_HEADS, SSD_HEAD_DIM)
        rep = SSD_HEADS // SSD_GROUPS
        bm = jnp.repeat(bm.reshape(b_, L, SSD_GROUPS, SSD_STATE), rep, axis=2)
        cm = jnp.repeat(cm.reshape(b_, L, SSD_GROUPS, SSD_STATE), rep, axis=2)
        dt = jax.nn.softplus(dt.reshape(b_, L, 2, SSD_HEADS).astype(jnp.float32) + dt_bias)
        log_a = -dt * jnp.exp(a_log.astype(jnp.float32))
        dirs = [(cm, bm, xs * dt[:, :, d, :, None], log_a[:, :, d]) for d in range(2)]
        return z, xs, dirs

    def finish(y, z, xs):
        b_, L = z.shape[0], z.shape[1]
        y = (y + d_skip[:, None] * xs).reshape(b_, L, D_SSD)
        return rms_norm(y * jax.nn.silu(z)) * norm_w

    z_c, xs_c, dirs_c = prep(p_c)
    z_l, xs_l, dirs_l = prep(p_l)
    y_c, y_l = bidir_two_stream(chunked_linear_scan, dirs_c, dirs_l)
    return finish(y_c, z_c, xs_c), finish(y_l, z_l, xs_l)


def retention_mixer(p_c, p_l, log_decay, norm_w, cos, sin):
    def prep(p, rotary):
        b_, L = p.shape[0], p.shape[1]
        q, k, v, g = jnp.split(p, 4, axis=-1)
        q, k, v = (t.reshape(b_, L, RET_HEADS, RET_HEAD_DIM) for t in (q, k, v))
        if rotary:
            q, k = apply_rope(q, cos, sin), apply_rope(k, cos, sin)
        k = k * RET_HEAD_DIM ** -0.5
        dirs = [(q, k, v, jnp.broadcast_to(log_decay[d], (b_, L, RET_HEADS))) for d in range(2)]
        return g, dirs

    def finish(y, g):
        b_, L = g.shape[0], g.shape[1]
        y = layer_norm(y).reshape(b_, L, D_RET) * norm_w
        return jax.nn.silu(g) * y

    g_c, dirs_c = prep(p_c, False)
    g_l, dirs_l = prep(p_l, True)
    y_c, y_l = bidir_two_stream(chunked_linear_scan, dirs_c, dirs_l)
    return finish(y_c, g_c), finish(y_l, g_l)


def gdn_mixer(p_c, p_l, conv_w, a_log, dt_bias, norm_w):
    def prep(p):
        b_, L = p.shape[0], p.shape[1]
        qkv, g, a_raw, b_raw = jnp.split(p, [3 * D_GDN, 4 * D_GDN, 4 * D_GDN + 2 * GDN_HEADS], axis=-1)
        qkv = jax.nn.silu(depthwise_conv(qkv, conv_w))
        q, k, v = (t.reshape(b_, L, GDN_HEADS, GDN_HEAD_DIM) for t in jnp.split(qkv, 3, axis=-1))
        q = l2_norm(q) * GDN_HEAD_DIM ** -0.5
        k = l2_norm(k)
        a_raw = a_raw.reshape(b_, L, 2, GDN_HEADS).astype(jnp.float32)
        log_a = -jnp.exp(a_log.astype(jnp.float32)) * jax.nn.softplus(a_raw + dt_bias)
        beta = jax.nn.sigmoid(b_raw.reshape(b_, L, 2, GDN_HEADS).astype(jnp.float32))
        dirs = [(q, k, v, beta[:, :, d], log_a[:, :, d]) for d in range(2)]
        return g, dirs

    def finish(y, g):
        b_, L = g.shape[0], g.shape[1]
        y = (rms_norm(y) * norm_w).reshape(b_, L, D_GDN)
        return jax.nn.silu(g) * y

    g_c, dirs_c = prep(p_c)
    g_l, dirs_l = prep(p_l)
    y_c, y_l = bidir_two_stream(chunked_delta_scan, dirs_c, dirs_l)
    return finish(y_c, g_c), finish(y_l, g_l)


def swiglu(h, w1, w3, w2):
    return (jax.nn.silu(h @ w1) * (h @ w3)) @ w2


def moe_swiglu(h, router, w1, w3, w2):
    logits = (h @ router).astype(jnp.float32)
    top_v, top_i = lax.top_k(logits, TOP_K)
    gates = jax.nn.softmax(top_v, axis=-1)
    dense_gate = jnp.sum(jax.nn.one_hot(top_i, N_EXPERTS, dtype=jnp.float32) * gates[..., None], axis=-2)
    y = jnp.zeros(h.shape, jnp.float32)
    for e in range(N_EXPERTS):
        y = y + dense_gate[..., e:e + 1] * swiglu(h, w1[e], w3[e], w2[e])
    return y.astype(h.dtype)


def channel_mixer(h, i, ffn_w1, ffn_w3, ffn_w2, moe_router, moe_w1, moe_w3, moe_w2):
    j = i // 2
    if i % 2 == 0:
        return swiglu(h, ffn_w1[j], ffn_w3[j], ffn_w2[j])
    return moe_swiglu(h, moe_router[j], moe_w1[j], moe_w3[j], moe_w2[j])


def setup_inputs(seed: int = 0) -> dict:
    key = jax.random.key(seed)
    ks = jax.random.split(key, 32)
    f32 = jnp.float32

    def nrm(i, shape, scale):
        return jax.random.normal(ks[i], shape, f32) * scale

    def gain(i, shape):
        return 1.0 + nrm(i, shape, 0.1)

    def a_log_init(i, shape):
        return jnp.log(jax.random.uniform(ks[i], shape, f32, 1.0, 16.0))

    def dt_bias_init(i, shape):
        dt = jnp.exp(jax.random.uniform(ks[i], shape, f32, math.log(1e-3), math.log(1e-1)))
        return dt + jnp.log(-jnp.expm1(-dt))

    u = jax.random.uniform(ks[13], (DEPTH, 2, RET_HEADS), f32)
    ret_log_decay = jnp.log1p(-jnp.exp2(-5.0 - jnp.arange(RET_HEADS, dtype=f32) - u))
    return {
        'x': nrm(0, (BATCH, SEQ, D_MODEL), 1.0),
        'c': nrm(1, (BATCH, D_MODEL), 1.0),
        'ctx': nrm(2, (BATCH, CTX_LEN, D_MODEL), 1.0),
        'c_ctx': nrm(3, (D_MODEL,), 1.0),
        'w_mod': nrm(4, (DEPTH, D_MODEL, 6 * D_MODEL), 0.5 * D_MODEL ** -0.5),
        'b_mod': nrm(5, (DEPTH, 6 * D_MODEL), 0.02),
        'w_in': nrm(6, (DEPTH, D_MODEL, N_IN_PROJ), D_MODEL ** -0.5),
        'ssd_conv_w': nrm(7, (DEPTH, CONV_W, SSD_XBC), CONV_W ** -0.5),
        'ssd_conv_b': nrm(8, (DEPTH, SSD_XBC), 0.02),
        'ssd_a_log': a_log_init(9, (DEPTH, 2, SSD_HEADS)),
        'ssd_dt_bias': dt_bias_init(10, (DEPTH, 2, SSD_HEADS)),
        'ssd_d': gain(11, (DEPTH, SSD_HEADS)),
        'ssd_norm_w': gain(12, (DEPTH, D_SSD)),
        'ret_log_decay': ret_log_decay,
        'ret_norm_w': gain(14, (DEPTH, D_RET)),
        'gdn_conv_w': nrm(15, (DEPTH, CONV_W, 3 * D_GDN), CONV_W ** -0.5),
        'gdn_a_log': a_log_init(16, (DEPTH, 2, GDN_HEADS)),
        'gdn_dt_bias': dt_bias_init(17, (DEPTH, 2, GDN_HEADS)),
        'gdn_norm_w': gain(18, (DEPTH, GDN_HEAD_DIM)),
        'w_out': nrm(19, (DEPTH, D_MIX, D_MODEL), DEEPNORM_BETA * D_MIX ** -0.5),
        'ln1_g': gain(20, (DEPTH, D_MODEL)),
        'ln1_b': nrm(21, (DEPTH, D_MODEL), 0.02),
        'ln2_g': gain(22, (DEPTH, D_MODEL)),
        'ln2_b': nrm(23, (DEPTH, D_MODEL), 0.02),
        'ffn_w1': nrm(24, (N_DENSE, D_MODEL, D_FF), D_MODEL ** -0.5),
        'ffn_w3': nrm(25, (N_DENSE, D_MODEL, D_FF), D_MODEL ** -0.5),
        'ffn_w2': nrm(26, (N_DENSE, D_FF, D_MODEL), DEEPNORM_BETA * D_FF ** -0.5),
        'moe_router': nrm(27, (N_MOE, D_MODEL, N_EXPERTS), D_MODEL ** -0.5),
        'moe_w1': nrm(28, (N_MOE, N_EXPERTS, D_MODEL, D_FF), D_MODEL ** -0.5),
        'moe_w3': nrm(29, (N_MOE, N_EXPERTS, D_MODEL, D_FF), D_MODEL ** -0.5),
        'moe_w2': nrm(30, (N_MOE, N_EXPERTS, D_FF, D_MODEL), DEEPNORM_BETA * D_FF ** -0.5),
    }


def reference(x, c, ctx, c_ctx, w_mod, b_mod, w_in, ssd_conv_w, ssd_conv_b, ssd_a_log, ssd_dt_bias,
              ssd_d, ssd_norm_w, ret_log_decay, ret_norm_w, gdn_conv_w, gdn_a_log, gdn_dt_bias, gdn_norm_w,
              w_out, ln1_g, ln1_b, ln2_g, ln2_b, ffn_w1, ffn_w3, ffn_w2, moe_router, moe_w1, moe_w3, moe_w2):
    rows = x.shape[1] // GRID_W
    cos, sin = rope_tables(rows, RET_HEAD_DIM)
    c_act = jax.nn.silu(c)
    cc_act = jax.nn.silu(c_ctx)
    x_l, x_c = x, ctx
    for i in range(DEPTH):
        last = i == DEPTH - 1
        mod_l = jnp.split((c_act @ w_mod[i] + b_mod[i])[:, None, :], 6, axis=-1)
        mod_c = jnp.split(cc_act @ w_mod[i] + b_mod[i], 6, axis=-1)
        p_c = modulate(x_c, mod_c[0], mod_c[1]) @ w_in[i]
        p_l = modulate(x_l, mod_l[0], mod_l[1]) @ w_in[i]
        ssd_c, ret_c, gdn_c = jnp.split(p_c, [SSD_COLS, SSD_COLS + RET_COLS], axis=-1)
        ssd_l, ret_l, gdn_l = jnp.split(p_l, [SSD_COLS, SSD_COLS + RET_COLS], axis=-1)
        ys_c, ys_l = ssd_mixer(ssd_c, ssd_l, ssd_conv_w[i], ssd_conv_b[i], ssd_a_log[i], ssd_dt_bias[i],
                               ssd_d[i], ssd_norm_w[i])
        yr_c, yr_l = retention_mixer(ret_c, ret_l, ret_log_decay[i], ret_norm_w[i], cos, sin)
        yg_c, yg_l = gdn_mixer(gdn_c, gdn_l, gdn_conv_w[i], gdn_a_log[i], gdn_dt_bias[i], gdn_norm_w[i])
        o_l = jnp.concatenate([ys_l, yr_l, yg_l], axis=-1) @ w_out[i]
        x_l = post_norm(x_l, mod_l[2] * o_l, ln1_g[i], ln1_b[i])
        f_l = channel_mixer(modulate(x_l, mod_l[3], mod_l[4]), i, ffn_w1, ffn_w3, ffn_w2,
                            moe_router, moe_w1, moe_w3, moe_w2)
        x_l = post_norm(x_l, mod_l[5] * f_l, ln2_g[i], ln2_b[i])
        if not last:
            o_c = jnp.concatenate([ys_c, yr_c, yg_c], axis=-1) @ w_out[i]
            x_c = post_norm(x_c, mod_c[2] * o_c, ln1_g[i], ln1_b[i])
            f_c = channel_mixer(modulate(x_c, mod_c[3], mod_c[4]), i, ffn_w1, ffn_w3, ffn_w2,
                                moe_router, moe_w1, moe_w3, moe_w2)
            x_c = post_norm(x_c, mod_c[5] * f_c, ln2_g[i], ln2_b[i])
    return x_l
```

```python
import numpy as np
import concourse.bass as bass
import concourse.mybir as mybir

F32 = mybir.dt.float32
BF16 = mybir.dt.bfloat16
AF = mybir.ActivationFunctionType
ALU = mybir.AluOpType
AX = mybir.AxisListType

COMPUTE = ("tensor", "vector", "scalar", "gpsimd")
ENGS = ("tensor", "vector", "scalar", "gpsimd", "sync")
NDSEM = 20


class Buf:
    def __init__(self, t, name, psum=False):
        self.t = t
        self.name = name
        self.psum = psum
        self.lastw = None
        self.readers = []

    def __getitem__(self, key):
        return View(self, self.t[key])

    def all(self):
        return View(self, self.t[:])


class View:
    def __init__(self, buf, ap):
        self.buf = buf
        self.ap = ap

    def __getitem__(self, key):
        return View(self.buf, self.ap[key])

    def bc(self, shape):
        return View(self.buf, self.ap.broadcast_to(shape))

    def un(self, axis):
        return View(self.buf, self.ap.unsqueeze(axis))

    def re(self, s, **kw):
        return View(self.buf, self.ap.rearrange(s, **kw))


class Ins:
    __slots__ = ("eng", "fn", "waits", "signal", "idx", "dma", "sig", "tag")

    def __init__(self, eng, fn):
        self.eng = eng
        self.fn = fn
        self.waits = []
        self.signal = False
        self.dma = None
        self.sig = 0


class Prog:
    def __init__(self, nc, stack):
        self.nc = nc
        self.stack = stack
        self.q = {e: [] for e in ENGS}
        self.known = {e: {p: -1 for p in ENGS} for e in ENGS}
        self.knownd = {e: {} for e in ENGS}
        self.dsem_tot = {}
        self.dcount = {e: 0 for e in ENGS}
        self.nbuf = 0
        self.esem = {e: stack.enter_context(nc.semaphore("es_" + e)) for e in COMPUTE}
        self.dsem = {}
        for e in ("sync", "gpsimd", "scalar"):
            for i in range(NDSEM):
                self.dsem[(e, i)] = stack.enter_context(nc.semaphore("ds_%s_%d" % (e, i)))
                self.dsem_tot[(e, i)] = 0
        self.outstanding = []

    def sb(self, name, shape, dt=F32, stack=None):
        st = stack or self.stack
        self.nbuf += 1
        t = st.enter_context(self.nc.sbuf_tensor("%s_%d" % (name, self.nbuf), list(shape), dt))
        return Buf(t, name)

    def ps(self, name, shape, dt=F32, stack=None):
        st = stack or self.stack
        self.nbuf += 1
        t = st.enter_context(self.nc.psum_tensor("%s_%d" % (name, self.nbuf), list(shape), dt))
        return Buf(t, name, psum=True)

    def dram(self, name, shape, dt=F32):
        t = self.nc.dram_tensor(name, list(shape), dt)
        return Buf(t, name)

    def _dep(self, ins, d, raw):
        if d is ins:
            return
        if d.dma is not None:
            sem, tot = d.dma
            if self.knownd[ins.eng].get(sem, 0) >= tot:
                return
            ins.waits.append(("d", sem, tot))
            self.knownd[ins.eng][sem] = tot
            return
        if d.eng == ins.eng and ins.dma is None:
            if d.eng == "tensor" or not raw:
                return
        if self.known[ins.eng][d.eng] >= d.idx:
            return
        d.signal = True
        ins.waits.append(("e", d.eng, d))
        self.known[ins.eng][d.eng] = d.idx

    def add(self, eng, fn, reads=(), writes=(), dma=False):
        ins = Ins(eng, fn)
        ins.tag = 'R:' + ','.join(getattr(v.buf if isinstance(v, View) else v, 'name', '?') for v in reads) + ' W:' + ','.join(getattr(v.buf if isinstance(v, View) else v, 'name', '?') for v in writes)
        ins.idx = len(self.q[eng])
        if dma:
            k = self.dcount[eng] % NDSEM
            self.dcount[eng] += 1
            sem = (eng, k)
            prev = self.dsem_tot[sem]
            if prev > self.knownd[eng].get(sem, 0):
                ins.waits.append(("d", sem, prev))
                self.knownd[eng][sem] = prev
            self.dsem_tot[sem] = prev + 16
            ins.dma = (sem, prev + 16)
        rb = [v.buf if isinstance(v, View) else v for v in reads]
        wb = [v.buf if isinstance(v, View) else v for v in writes]
        for b in rb:
            if b.lastw is not None:
                self._dep(ins, b.lastw, True)
            if b.psum:
                for r in b.readers:
                    if r.eng != eng:
                        self._dep(ins, r, False)
        for b in wb:
            if b.lastw is not None:
                self._dep(ins, b.lastw, False)
            for r in b.readers:
                self._dep(ins, r, False)
        for b in wb:
            b.lastw = ins
            b.readers = []
        for b in rb:
            if b not in wb:
                b.readers.append(ins)
        self.q[eng].append(ins)
        return ins

    def dma(self, q, out, in_, **kw):
        o, i = out.ap, in_.ap
        return self.add(q, lambda e: e.dma_start(out=o, in_=i, **kw), [in_], [out], dma=True)

    def mm(self, out, lhsT, rhs, start=True, stop=True):
        o, l, r = out.ap, lhsT.ap, rhs.ap
        rd = [lhsT, rhs] if start else [lhsT, rhs, out]
        return self.add("tensor", lambda e: e.matmul(o, l, r, start=start, stop=stop), rd, [out])

    def tr(self, out, in_, ident):
        o, i, d = out.ap, in_.ap, ident.ap
        return self.add("tensor", lambda e: e.transpose(o, i, d), [in_, ident], [out])

    def act(self, out, in_, func, bias=0.0, scale=1.0, eng="scalar"):
        o, i = out.ap, in_.ap
        rd = [in_]
        b, s = bias, scale
        if isinstance(bias, View):
            rd.append(bias)
            b = bias.ap
        if isinstance(scale, View):
            rd.append(scale)
            s = scale.ap
        return self.add("scalar", lambda e: e.activation(out=o, in_=i, func=func, bias=b, scale=s), rd, [out])

    def tt(self, out, in0, in1, op, eng="vector"):
        if in0.buf.psum:
            return self.stt(out, in0, 1.0, in1, ALU.mult, op, eng=eng)
        if in1.buf.psum:
            if op == ALU.subtract:
                return self.stt(out, in1, -1.0, in0, ALU.mult, ALU.add, eng=eng)
            return self.stt(out, in1, 1.0, in0, ALU.mult, op, eng=eng)
        o, a, b = out.ap, in0.ap, in1.ap
        return self.add(eng, lambda e: e.tensor_tensor(out=o, in0=a, in1=b, op=op), [in0, in1], [out])

    def ts(self, out, in0, s1, s2=None, op0=ALU.mult, op1=None, eng="vector"):
        o, a = out.ap, in0.ap
        rd = [in0]
        x1, x2 = s1, s2
        if isinstance(s1, View):
            rd.append(s1)
            x1 = s1.ap
        if isinstance(s2, View):
            rd.append(s2)
            x2 = s2.ap
        if op1 is None:
            return self.add(eng, lambda e: e.tensor_scalar(out=o, in0=a, scalar1=x1, scalar2=None, op0=op0), rd, [out])
        return self.add(eng, lambda e: e.tensor_scalar(out=o, in0=a, scalar1=x1, scalar2=x2, op0=op0, op1=op1), rd, [out])

    def stt(self, out, in0, scalar, in1, op0, op1, eng="vector"):
        o, a, b = out.ap, in0.ap, in1.ap
        rd = [in0, in1]
        s = scalar
        if isinstance(scalar, View):
            rd.append(scalar)
            s = scalar.ap
        return self.add(eng, lambda e: e.scalar_tensor_tensor(out=o, in0=a, scalar=s, in1=b, op0=op0, op1=op1), rd, [out])

    def copy(self, out, in_, eng="vector"):
        o, i = out.ap, in_.ap
        if eng == "scalar":
            return self.add(eng, lambda e: e.copy(out=o, in_=i), [in_], [out])
        return self.add(eng, lambda e: e.tensor_copy(out=o, in_=i), [in_], [out])

    def rsum(self, out, in_, eng="vector"):
        o, i = out.ap, in_.ap
        return self.add(eng, lambda e: e.reduce_sum(out=o, in_=i, axis=AX.X), [in_], [out])

    def rmax(self, out, in_, eng="vector"):
        o, i = out.ap, in_.ap
        return self.add(eng, lambda e: e.reduce_max(out=o, in_=i, axis=AX.X), [in_], [out])

    def memset(self, out, val, eng="vector"):
        o = out.ap
        return self.add(eng, lambda e: e.memset(o, val), [], [out])

    def recip(self, out, in_):
        o, i = out.ap, in_.ap
        return self.add("vector", lambda e: e.reciprocal(out=o, in_=i), [in_], [out])

    def barrier(self, scratch):
        hub = "vector"
        ins = self.add(hub, (lambda o: (lambda e: e.memset(o, 0.0)))(scratch.ap), [], [])
        for e in ENGS:
            if e == hub or not self.q[e]:
                continue
            last = None
            for x in reversed(self.q[e]):
                if x.dma is None and x.fn is not None:
                    last = x
                    break
            if last is not None and e in COMPUTE:
                if self.known[hub][e] < last.idx:
                    last.signal = True
                    ins.waits.append(("e", e, last))
                    self.known[hub][e] = last.idx
        for sem, tot in self.dsem_tot.items():
            if tot > self.knownd[hub].get(sem, 0):
                ins.waits.append(("d", sem, tot))
                self.knownd[hub][sem] = tot
        ins.signal = True
        for e in ENGS:
            if e == hub:
                continue
            w = self.add(e, None, [], [])
            w.waits.append(("e", hub, ins))
            self.known[e][hub] = ins.idx
            for sem, tot in self.dsem_tot.items():
                self.knownd[e][sem] = max(self.knownd[e].get(sem, 0), tot)
            for p in ENGS:
                if self.q[p]:
                    self.known[e][p] = max(self.known[e][p], len(self.q[p]) - 1 if p != e else -1)
        return ins

    def dump(self, path):
        for e in COMPUTE:
            c = 0
            for ins in self.q[e]:
                if ins.signal:
                    c += 1
                ins.sig = c
        with open(path, "w") as f:
            for e in ENGS:
                f.write("==== %s\n" % e)
                for ins in self.q[e]:
                    ws = " ".join(("d%s>=%d" % (w[1], w[2])) if w[0] == "d" else ("%s>=%d(i%d)" % (w[1], w[2].sig, w[2].idx)) for w in ins.waits)
                    f.write("%5d %s%s %s | %s\n" % (ins.idx, "S%d " % ins.sig if ins.signal else "", "DMA%s" % (ins.dma,) if ins.dma else "", ins.tag, ws))

    def emit(self):
        for e in COMPUTE:
            c = 0
            for ins in self.q[e]:
                if ins.signal:
                    c += 1
                ins.sig = c
        P = self

        def run(eng_name):
            def f(e):
                for ins in P.q[eng_name]:
                    for w in ins.waits:
                        if w[0] == "d":
                            e.wait_ge(P.dsem[w[1]], w[2])
                        else:
                            e.wait_ge(P.esem[w[1]], w[2].sig)
                    if ins.fn is None:
                        continue
                    r = ins.fn(e)
                    if ins.dma is not None:
                        r.then_inc(P.dsem[ins.dma[0]], 16)
                    elif ins.signal:
                        r.then_inc(P.esem[eng_name], 1)
            return f

        with self.nc.Block() as block:
            block.sync(run("sync"))
            block.scalar(run("scalar"))
            block.vector(run("vector"))
            block.gpsimd(run("gpsimd"))
            block.tensor(run("tensor"))


from contextlib import ExitStack
import numpy as np

D = 2048
NCOL = 1804
PW = 2208
EPS = 1e-6
BIG = 30000.0
C_CT, C_BT, C_BTOK, C_XS, C_Z = 0, 128, 256, 384, 640
C_RQT, C_RKT, C_RKTOK, C_RV, C_RG = 896, 1024, 1152, 1280, 1408
C_GQT, C_GKT, C_GKTOK, C_GV, C_GG = 1536, 1664, 1792, 1920, 2048
C_SM = 2176
RK = 282
import os
STAGE = int(os.environ.get('KB_STAGE', '9'))
P2 = int(os.environ.get('KB_P2', '9'))
CUT = int(os.environ.get('KB_CUT', '99'))
SKIP = os.environ.get('KB_SKIP', '').split(',')


def build_B(NT, npass=3):
    NTC = 2
    nc = bass.Bass("TRN2", target_bir_lowering=False)
    dx = nc.dram_tensor("x", [NT, 128, D], F32, kind="ExternalInput")
    dmod = nc.dram_tensor("modc", [128, 4, 16], F32, kind="ExternalInput")
    dwin = nc.dram_tensor("win", [D, NCOL], F32, kind="ExternalInput")
    dcw = nc.dram_tensor("cw", [128, 7, 5], F32, kind="ExternalInput")
    dcb = nc.dram_tensor("cb", [128, 7], F32, kind="ExternalInput")
    drow = nc.dram_tensor("rowp", [1, RK], F32, kind="ExternalInput")
    dconst = nc.dram_tensor("consts", [8, 128, 128], F32, kind="ExternalInput")
    drope = nc.dram_tensor("rope", [NT, 128, 256], F32, kind="ExternalInput")
    dy = nc.dram_tensor("yout", [NT, 128, 512], F32, kind="ExternalOutput")
    with ExitStack() as st:
        P = Prog(nc, st)
        X = Buf(dx, "x"); MODC = Buf(dmod, "modc"); WIN = Buf(dwin, "win"); CW = Buf(dcw, "cw"); CB = Buf(dcb, "cb")
        ROW = Buf(drow, "rowp"); CONST = Buf(dconst, "consts"); ROPE = Buf(drope, "rope"); YOUT = Buf(dy, "yout")
        PREP = [P.dram("prep%d" % t, [128, PW]) for t in range(NT)]
        YDIR = [[P.dram("ydir%d_%d" % (d, t), [128, 512]) for t in range(NT)] for d in range(2)]
        cst = P.sb("cst", [128, 8, 128])
        P.dma("sync", cst.all(), CONST.all().re("c p n -> p c n"))
        IDENT, ONES = cst[:, 0, :], cst[:, 1, :]
        TRI = [cst[:, 2, :], cst[:, 3, :]]
        NEG = [cst[:, 4, :], cst[:, 5, :]]
        POS = [cst[:, 6, :], cst[:, 7, :]]
        rowb = P.sb("rowb", [128, RK])
        P.dma("sync", rowb.all(), View(ROW, ROW.t[0:1, :].broadcast_to([128, RK])))
        scr = P.sb("scr", [128, 8])
        negA = P.sb("negA", [128, 10])
        if 'negA' not in SKIP:
            P.act(negA.all(), rowb[:, 10:20], AF.Exp)
            P.ts(negA.all(), negA.all(), -1.0)
        ps_pool = [P.ps("ps%d" % i, [128, 512]) for i in range(8)]
        psi = [0]

        def PS():
            b = ps_pool[psi[0] % 8]
            psi[0] += 1
            return b

        class Pool:
            def __init__(s, name, shape, n, dt=F32, stack=None):
                s.b = [P.sb(name + str(i), shape, dt, stack) for i in range(n)]
                s.i = 0

            def next(s):
                b = s.b[s.i % len(s.b)]
                s.i += 1
                return b

        with ExitStack() as s1:
            wsb = P.sb("wsb", [128, 16, NCOL], BF16, s1)
            for k in range(16 if 'wsb' not in SKIP else 0):
                P.dma("gpsimd", wsb[:, k, :], WIN[k * 128:(k + 1) * 128, :])
            modc = P.sb("modc", [128, 4, 16], F32, s1)
            P.dma("sync", modc.all(), MODC.all())
            if 'modc' not in SKIP:
                P.ts(modc[:, 0, :], modc[:, 0, :], 1.0, op0=ALU.add)
                P.ts(modc[:, 2, :], modc[:, 2, :], 1.0, op0=ALU.add)
            cw = P.sb("cw", [128, 7, 5], F32, s1)
            if 'cw' not in SKIP:
                P.dma("sync", cw.all(), CW.all())
            cb = P.sb("cb", [128, 7], F32, s1)
            if 'cb' not in SKIP:
                P.dma("sync", cb.all(), CB.all())
            xp = Pool("xt", [128, D], 2, F32, s1)
            sqp = Pool("sq", [128, D], 1, BF16, s1)
            xhp = Pool("xh", [128, D], 2, F32, s1)
            stp = Pool("stt", [128, 16], 4, F32, s1)
            hTp = Pool("hT", [128, 16, 128], 2, BF16, s1)
            Up = Pool("U", [128, 7, 132], 3, F32, s1)
            PTp = Pool("PT", [128, PW], 2, F32, s1)
            for _b in PTp.b:
                P.memset(_b[:, C_SM + 20:PW], 0.0)
            tmA = Pool("tmA", [128, 396], 2, F32, s1)
            accp = Pool("cacc", [128, 7, 128], 2, F32, s1)
            ctp = Pool("ctmp", [128, 7, 128], 1, F32, s1)
            cvp = Pool("cv", [128, 7, 128], 2, F32, s1)
            rp = Pool("ropeT", [128, 256], 2, F32, s1)
            qkp = Pool("qk", [128, 512], 2, F32, s1)
            t64 = Pool("t64", [128, 4, 64], 2, F32, s1)
            sqb = Pool("sqb", [128, 256], 2, F32, s1)
            Ubufs = {}
            PTs = {}
            tmAs = {}

            def stream_first(t):
                return t == 0 or t == NTC

            def stream_last(t):
                return t == NTC - 1 or t == NT - 1

            def proj(t):
                if STAGE < 1:
                    return
                xt = xp.next()
                P.dma("sync", xt.all(), X[t])
                sq = sqp.next()
                P.act(sq.all(), xt.all(), AF.Square)
                s = stp.next()
                P.rsum(s[:, 0:1], xt.all())
                P.rsum(s[:, 1:2], sq.all())
                P.ts(s[:, 2:3], s[:, 0:1], 1.0 / D)
                P.tt(s[:, 3:4], s[:, 2:3], s[:, 2:3], ALU.mult)
                P.stt(s[:, 4:5], s[:, 1:2], 1.0 / D, s[:, 3:4], ALU.mult, ALU.subtract)
                P.act(s[:, 5:6], s[:, 4:5], AF.Ln, bias=EPS); P.act(s[:, 5:6], s[:, 5:6], AF.Exp, scale=-0.5)
                P.stt(s[:, 6:7], s[:, 2:3], -1.0, s[:, 5:6], ALU.mult, ALU.mult)
                xh = xhp.next()
                P.act(xh.all(), xt.all(), AF.Identity, bias=s[:, 6:7], scale=s[:, 5:6])
                if STAGE < 2:
                    return
                hT = hTp.next()
                mi = 2 if t < NTC else 0
                for kb in range(4):
                    ps = PS()
                    for j in range(4):
                        k = kb * 4 + j
                        P.tr(ps[:, j * 128:(j + 1) * 128], xh[:, k * 128:(k + 1) * 128], IDENT)
                    for j in range(4):
                        k = kb * 4 + j
                        if True:
                            P.act(hT[:, k, :], ps[:, j * 128:(j + 1) * 128], AF.Identity, bias=modc[:, mi + 1, k:k + 1], scale=modc[:, mi, k:k + 1])
                if STAGE < 3:
                    return
                U = Up.next()
                Ubufs[t] = U
                if stream_first(t):
                    P.memset(U[:, :, 0:2], 0.0)
                if stream_last(t):
                    P.memset(U[:, :, 130:132], 0.0)
                for half in range(2):
                    nb = 4 if half == 0 else 3
                    ps = PS()
                    for j in range(nb):
                        b = half * 4 + j
                        for k in range(16):
                            P.mm(ps[:, j * 128:(j + 1) * 128], wsb[:, k, b * 128:(b + 1) * 128], hT[:, k, :], start=(k == 0), stop=(k == 15))
                    P.copy(U[:, half * 4:half * 4 + nb, 2:130], ps[:, 0:nb * 128].re("p (b n) -> p b n", n=128), eng="scalar")
                if not stream_first(t):
                    Uprev = Ubufs[t - 1]
                    P.copy(Uprev[:, :, 130:132], U[:, :, 2:4])
                    P.copy(U[:, :, 0:2], Uprev[:, :, 128:130])
                if STAGE < 4:
                    return
                PT = PTp.next()
                PTs[t] = PT
                psa = PS()
                for k in range(16):
                    P.mm(psa[:, 0:396], hT[:, k, :], wsb[:, k, 896:1292], start=(k == 0), stop=(k == 15))
                ta = tmA.next()
                tmAs[t] = ta
                P.copy(ta.all(), psa[:, 0:396], eng="scalar")
                psb = PS()
                for k in range(16):
                    P.mm(psb[:, 0:512], hT[:, k, :], wsb[:, k, 1292:1804], start=(k == 0), stop=(k == 15))
                P.copy(PT[:, C_Z:C_Z + 256], ta[:, 0:256])
                P.copy(PT[:, C_GG:C_GG + 128], ta[:, 268:396])
                s10 = stp.next()
                s10b = stp.next()
                P.tt(scr_v(s10, 10), ta[:, 256:266], rowb[:, 0:10], ALU.add)
                P.act(scr_v(s10b, 10), scr_v(s10, 10), AF.Exp)
                P.act(scr_v(s10, 10), scr_v(s10b, 10), AF.Ln, bias=1.0)
                P.copy(PT[:, C_SM:C_SM + 8], s10t(s10)[:, 0:8])
                P.tt(PT[:, C_SM + 8:C_SM + 18], scr_v(s10, 10), negA.all(), ALU.mult)
                P.act(PT[:, C_SM + 18:C_SM + 20], ta[:, 266:268], AF.Sigmoid)
                if STAGE < 5:
                    return
                rt = rp.next()
                P.dma("sync", rt.all(), ROPE[t])
                qk = qkp.next()
                P.copy(qk.all(), psb[:, 0:512], eng="scalar")
                P.copy(PT[:, C_RV:C_RV + 128], qk[:, 256:384])
                P.copy(PT[:, C_RG:C_RG + 128], qk[:, 384:512])
                rot = qkp.next()
                tmp = t64.next()
                for i, (src, cc, dst) in enumerate(((0, 0, None), (128, 128, C_RKTOK))):
                    x1, x2 = qk[:, src:src + 64], qk[:, src + 64:src + 128]
                    co, si = rt[:, cc:cc + 64], rt[:, cc + 64:cc + 128]
                    o1 = rot[:, src:src + 64] if dst is None else PT[:, dst:dst + 64]
                    o2 = rot[:, src + 64:src + 128] if dst is None else PT[:, dst + 64:dst + 128]
                    P.tt(tmp[:, 0, :], x1, co, ALU.mult)
                    P.tt(tmp[:, 1, :], x2, si, ALU.mult)
                    P.tt(o1, tmp[:, 0, :], tmp[:, 1, :], ALU.subtract)
                    P.tt(tmp[:, 2, :], x2, co, ALU.mult)
                    P.tt(tmp[:, 3, :], x1, si, ALU.mult)
                    P.tt(o2, tmp[:, 2, :], tmp[:, 3, :], ALU.add)
                ps = PS()
                P.tr(ps[:, 0:128], rot[:, 0:128], IDENT)
                P.tr(ps[:, 128:256], PT[:, C_RKTOK:C_RKTOK + 128], IDENT)
                P.copy(PT[:, C_RQT:C_RQT + 256], ps[:, 0:256], eng="scalar")

            def scr_v(b, n):
                return b[:, 0:n]

            def s10t(b):
                return b

            def conv(t):
                if STAGE < 6:
                    return
                U = Ubufs[t]
                PT = PTs[t]
                acc = accp.next()
                ct = ctp.next()
                for i in range(5):
                    wv = cw[:, :, i:i + 1].bc([128, 7, 128])
                    if i == 0:
                        P.tt(acc.all(), U[:, :, 0:128], wv, ALU.mult)
                    else:
                        P.tt(ct.all(), U[:, :, i:i + 128], wv, ALU.mult, eng="gpsimd")
                        P.tt(acc.all(), acc.all(), ct.all(), ALU.add)
                cv = cvp.next()
                for b in range(7):
                    P.act(cv[:, b, :], acc[:, b, :], AF.Silu, bias=cb[:, b:b + 1])
                P.copy(PT[:, C_BT:C_BT + 128], cv[:, 2, :])
                P.copy(PT[:, C_CT:C_CT + 128], cv[:, 3, :])
                ps = PS()
                P.tr(ps[:, 0:128], cv[:, 2, :], IDENT)
                P.tr(ps[:, 128:256], cv[:, 0, :], IDENT)
                P.tr(ps[:, 256:384], cv[:, 1, :], IDENT)
                P.tr(ps[:, 384:512], cv[:, 6, :], IDENT)
                P.copy(PT[:, C_BTOK:C_BTOK + 384], ps[:, 0:384], eng="scalar")
                P.copy(PT[:, C_GV:C_GV + 128], ps[:, 384:512])
                sq = sqb.next()
                P.act(sq.all(), cv[:, 4:6, :].re("p b n -> p (b n)"), AF.Square)
                ps2 = PS()
                P.mm(ps2[:, 0:256], ONES, sq.all())
                rs = sqb.next()
                P.act(rs.all(), ps2[:, 0:256], AF.Ln, bias=EPS); P.act(rs.all(), rs.all(), AF.Exp, scale=-0.5)
                P.stt(PT[:, C_GQT:C_GQT + 128], cv[:, 4, :], 128.0 ** -0.5, rs[:, 0:128], ALU.mult, ALU.mult)
                P.tt(PT[:, C_GKT:C_GKT + 128], cv[:, 5, :], rs[:, 128:256], ALU.mult)
                ps3 = PS()
                P.tr(ps3[:, 0:128], PT[:, C_GKT:C_GKT + 128], IDENT)
                P.copy(PT[:, C_GKTOK:C_GKTOK + 128], ps3[:, 0:128], eng="scalar")
                P.dma("sync", PREP[t].all(), PT.all())

            for t in range(NT):
                proj(t)
                if not stream_first(t):
                    conv(t - 1)
                if stream_last(t):
                    conv(t)
        if npass >= 2:
            P.barrier(scr[:, 0:1])
            pass2(P, NT, NTC, PREP, YDIR, rowb, IDENT, ONES, TRI, NEG, POS, PS, Pool, lambda i: ps_pool[i])
        if npass >= 3:
            P.barrier(scr[:, 0:1])
            pass3(P, NT, PREP, YDIR, YOUT, rowb, Pool)
        else:
            P.barrier(scr[:, 0:1])
            with ExitStack() as s9:
                pp = Pool("dbg", [128, 512], 2, F32, s9)
                for t in range(NT if 'dbg' not in SKIP else 0):
                    b = pp.next()
                    if npass == 1:
                        P.dma("sync", b.all(), PREP[t][:, 0:512])
                    else:
                        P.dma("sync", b.all(), YDIR[0][t].all())
                    P.dma("sync", YOUT[t], b.all())
        P.barrier(scr[:, 0:1])
        if os.environ.get('KB_DUMP'):
            P.dump(os.environ['KB_DUMP'])
        P.emit()
    return nc


def pass2(P, NT, NTC, PREP, YDIR, rowb, IDENT, ONES, TRI, NEG, POS, PS, Pool, PSB):
    with ExitStack() as s2:
        neg4 = [P.sb("neg4_%d" % d, [128, 6, 128], F32, s2) for d in range(2)]
        for d in range(2):
            for h in range(6):
                P.copy(neg4[d][:, h, :], NEG[d])
        PTp = Pool("PT2", [128, PW], 3, F32, s2)
        barscr = P.sb("barscr", [128, 8], F32, s2)
        a6p = Pool("a6", [128, 8], 3, F32, s2)
        g12p = Pool("g12", [128, 12], 3, F32, s2)
        e12p = Pool("e12", [128, 12], 3, F32, s2)
        ekp = Pool("ek", [128, 8], 3, F32, s2)
        nbp = Pool("nb", [128, 8], 3, F32, s2)
        Rp = Pool("R", [128, 6, 128], 2, F32, s2)
        decp = Pool("dec", [128, 7, 128], 2, F32, s2)
        MTp = Pool("MT", [128, 4, 128], 2, F32, s2)
        vp = Pool("v", [128, 4, 64], 2, F32, s2)
        kdp = Pool("kd", [128, 4, 128], 2, F32, s2)
        m1p = Pool("m1", [128, 128], 6, F32, s2)
        mrp = Pool("mr", [128, 128], 2, F32, s2)
        atp = Pool("at", [128, 128], 2, F32, s2)
        krp = Pool("kr", [128, 128], 2, F32, s2)
        wtp = Pool("wt", [128, 128], 2, F32, s2)
        vnp = Pool("vn", [128, 128], 2, F32, s2)
        kgp = Pool("kg", [128, 128], 2, F32, s2)
        ysp = Pool("ysb", [128, 512], 2, F32, s2)
        yibp = Pool("yib", [128, 256], 2, F32, s2)
        yop = Pool("yo", [128, 512], 2, F32, s2)
        xp = Pool("Xs", [128, 256], 4, F32, s2)
        Ss = [[P.sb("S%d_%d" % (d, i), [128, 512], F32, s2) for i in range(2)] for d in range(2)]
        for d in range(2):
            P.memset(Ss[d][0].all(), 0.0)
        orders = [list(range(NT)), list(range(NTC - 1, -1, -1)) + list(range(NT - 1, NTC - 1, -1))]
        for step in range(NT):
            for d in range(2):
                t = orders[d][step]
                S, S2 = Ss[d][step % 2], Ss[d][(step + 1) % 2]
                if 'itbar' in SKIP:
                    P.barrier(rowb[:, RK - 1:RK]) if False else P.barrier(barscr[:, 0:1])
                PT = PTp.next()
                P.dma("sync", PT.all(), PREP[t].all())
                a6 = a6p.next()
                P.copy(a6[:, 0:4], PT[:, C_SM + 8 + d * 4:C_SM + 12 + d * 4])
                P.copy(a6[:, 4:5], rowb[:, 24 + d:25 + d])
                P.copy(a6[:, 5:6], PT[:, C_SM + 16 + d:C_SM + 17 + d])
                psg = PSB(0)
                P.mm(psg[:, 0:6], TRI[d], a6[:, 0:6])
                P.mm(psg[:, 6:12], ONES, a6[:, 0:6])
                g12 = g12p.next()
                P.copy(g12.all(), psg[:, 0:12], eng="scalar")
                e12 = e12p.next()
                P.act(e12.all(), g12.all(), AF.Exp)
                ek = ekp.next()
                if step < NT - 1:
                    P.tt(ek[:, 0:6], g12[:, 6:12], g12[:, 0:6], ALU.subtract)
                    P.act(ek[:, 0:6], ek[:, 0:6], AF.Exp)
                nb = nbp.next()
                P.ts(nb[:, 0:6], g12[:, 0:6], -1.0)
                P.ts(nb[:, 6:7], PT[:, C_SM + 18 + d:C_SM + 19 + d], -1.0)
                P.tt(nb[:, 7:8], PT[:, C_SM + 18 + d:C_SM + 19 + d], e12[:, 5:6], ALU.mult)
                if P2 < 1:
                    continue
                R = Rp.next()
                P.tt(R.all(), a6[:, 0:6].un(2).bc([128, 6, 128]), TRI[d].un(1).bc([128, 6, 128]), ALU.mult)
                if 'mmA' in SKIP:
                    continue
                psA, psB = PSB(1), PSB(2)
                P.mm(psA[:, 0:512], ONES, R[:, 0:4, :].re("p h n -> p (h n)"), start=True, stop=False)
                P.mm(psA[:, 0:512], IDENT, neg4[d][:, 0:4, :].re("p h n -> p (h n)"), start=False, stop=True)
                P.mm(psB[:, 0:256], ONES, R[:, 4:6, :].re("p h n -> p (h n)"), start=True, stop=False)
                P.mm(psB[:, 0:256], IDENT, neg4[d][:, 4:6, :].re("p h n -> p (h n)"), start=False, stop=True)
                P.mm(psB[:, 256:384], ONES, R[:, 5, :], start=True, stop=False)
                P.mm(psB[:, 256:384], IDENT, POS[d], start=False, stop=True)
                dec = decp.next()
                if 'dec' in SKIP:
                    continue
                for h in range(4 if 'decA' not in SKIP else 0):
                    P.act(dec[:, h, :], psA[:, h * 128:(h + 1) * 128], AF.Exp, bias=nb[:, h:h + 1])
                for h in range(4, 6 if 'decB' not in SKIP else 4):
                    P.act(dec[:, h, :], psB[:, (h - 4) * 128:(h - 3) * 128], AF.Exp, bias=nb[:, h:h + 1])
                if 'dec2' not in SKIP:
                    P.act(dec[:, 6, :], psB[:, 256:384], AF.Exp, bias=g12[:, 5:6], scale=-1.0)
                if P2 < 2:
                    continue
                pss = PSB(3)
                P.mm(pss[:, 0:128], PT[:, C_BT:C_BT + 128], PT[:, C_CT:C_CT + 128])
                P.mm(pss[:, 128:256], PT[:, C_RKT:C_RKT + 128], PT[:, C_RQT:C_RQT + 128])
                P.mm(pss[:, 256:384], PT[:, C_GKT:C_GKT + 128], PT[:, C_GQT:C_GQT + 128])
                if 'nokk' not in SKIP:
                    P.mm(pss[:, 384:512], PT[:, C_GKT:C_GKT + 128], PT[:, C_GKT:C_GKT + 128])
                if 'extra' in SKIP:
                    P.mm(pss[:, 0:128], PT[:, C_BT:C_BT + 128], PT[:, C_CT:C_CT + 128])
                    P.mm(pss[:, 128:256], PT[:, C_RKT:C_RKT + 128], PT[:, C_RQT:C_RQT + 128])
                if 'nomt' in SKIP:
                    continue
                MT = MTp.next()
                if True:
                    for h in range(4):
                        P.tt(MT[:, h, :], pss[:, 0:128], dec[:, h, :], ALU.mult)
                else:
                    P.tt(MT.all(), pss[:, 0:128].un(1).bc([128, 4, 128]), dec[:, 0:4, :], ALU.mult)
                MR = mrp.next()
                if 'noMR' not in SKIP:
                  P.tt(MR.all(), pss[:, 128:256], dec[:, 4, :], ALU.mult)
                AT = atp.next()
                if 'noAT' not in SKIP:
                  P.tt(AT.all(), pss[:, 256:384], dec[:, 5, :], ALU.mult)
                Pk = m1p.next()
                if 'noPk' in SKIP:
                    pass
                elif 'pksplit' in SKIP:
                    P.tt(Pk.all(), pss[:, 384:512], dec[:, 6, :], ALU.mult)
                    P.ts(Pk.all(), Pk.all(), nb[:, 6:7])
                else:
                    P.stt(Pk.all(), pss[:, 384:512], nb[:, 6:7], dec[:, 6, :], ALU.mult, ALU.mult)
                if P2 < 3:
                    continue
                v = vp.next()
                P.tt(v.all(), PT[:, C_XS:C_XS + 256].re("p (h q) -> p h q", q=64),
                     PT[:, C_SM + d * 4:C_SM + d * 4 + 4].un(2).bc([128, 4, 64]), ALU.mult)
                psy = PSB(4)
                for h in range(4):
                    P.mm(psy[:, h * 64:(h + 1) * 64], MT[:, h, :], v[:, h, :])
                P.mm(psy[:, 256:512], PT[:, C_CT:C_CT + 128], S[:, 0:256])
                psr = PSB(5)
                P.mm(psr[:, 0:128], MR.all(), PT[:, C_RV:C_RV + 128])
                P.mm(psr[:, 128:256], PT[:, C_RQT:C_RQT + 128], S[:, 256:384])
                if CUT < 1:
                    continue
                ysb = ysp.next()
                P.copy(ysb[:, 0:256], psy[:, 0:256], eng="scalar")
                P.copy(ysb[:, 256:384], psr[:, 0:128], eng="scalar")
                yo = yop.next()
                tmpy = xp.next()
                yib = yibp.next()
                P.copy(yib.all(), psy[:, 256:512], eng="scalar")
                P.tt(tmpy.all().re("p (h q) -> p h q", q=64), yib.all().re("p (h q) -> p h q", q=64),
                     e12[:, 0:4].un(2).bc([128, 4, 64]), ALU.mult)
                P.tt(yo[:, 0:256], tmpy.all(), ysb[:, 0:256], ALU.add)
                P.stt(yo[:, 256:384], psr[:, 128:256], e12[:, 4:5], ysb[:, 256:384], ALU.mult, ALU.add)
                if step < NT - 1:
                    if CUT < 2:
                        pass
                    kd = kdp.next()
                    P.tt(kd.all(), PT[:, C_BTOK:C_BTOK + 128].un(1).bc([128, 4, 128]), ek[:, 0:4].un(2).bc([128, 4, 128]), ALU.mult)
                    kr = krp.next()
                    P.ts(kr.all(), PT[:, C_RKTOK:C_RKTOK + 128], ek[:, 4:5])
                    if CUT < 3:
                        pass
                    psc = psr if 'pscR' in SKIP else PSB(6)
                    for h in range(4 if 'nopscS' not in SKIP else 0):
                        P.mm(psc[:, (256 if 'pscR' in SKIP else 0) + h * 64:(256 if 'pscR' in SKIP else 0) + (h + 1) * 64], (MT if 'dupMT' in SKIP else kd)[:, h, :], v[:, h, :])
                    if 'nopscR' not in SKIP:
                        P.mm(psc[:, 256:384], kr.all(), PT[:, C_RV:C_RV + 128])
                    if CUT < 4:
                        pass
                    for h in range(4):
                        P.ts(S2[:, h * 64:(h + 1) * 64], S[:, h * 64:(h + 1) * 64], e12[:, 6 + h:7 + h])
                        P.tt(S2[:, h * 64:(h + 1) * 64], psc[:, h * 64:(h + 1) * 64], S2[:, h * 64:(h + 1) * 64], ALU.add)
                    P.ts(S2[:, 256:384], S[:, 256:384], e12[:, 10:11])
                    P.tt(S2[:, 256:384], psc[:, 256:384], S2[:, 256:384], ALU.add)
                if P2 < 4:
                    continue
                pst = PSB(0)
                P.tr(pst[:, 0:128], Pk.all(), IDENT)
                PTk = m1p.next()
                P.copy(PTk.all(), pst[:, 0:128], eng="scalar")
                Xc = xp.next()
                P.ts(Xc[:, 0:128], PT[:, C_GV:C_GV + 128], PT[:, C_SM + 18 + d:C_SM + 19 + d])
                P.ts(Xc[:, 128:256], PT[:, C_GKTOK:C_GKTOK + 128], nb[:, 7:8])
                for lev in range(7):
                    psx = PSB(7)
                    P.mm(psx[:, 0:256], PTk.all(), Xc.all())
                    if lev < 5:
                        P.mm(psx[:, 256:384], PTk.all(), Pk.all())
                    if lev < 6:
                        P.mm(psx[:, 384:512], Pk.all(), PTk.all())
                    Xn = xp.next()
                    P.tt(Xn.all(), psx[:, 0:256], Xc.all(), ALU.add)
                    Xc = Xn
                    if lev < 6:
                        Pn, PTn = m1p.next(), m1p.next()
                        if lev < 5:
                            P.copy(Pn.all(), psx[:, 256:384], eng="scalar")
                        P.copy(PTn.all(), psx[:, 384:512], eng="scalar")
                        Pk, PTk = Pn, PTn
                psw = PSB(0)
                P.tr(psw[:, 0:128], Xc[:, 128:256], IDENT)
                wT = wtp.next()
                P.copy(wT.all(), psw[:, 0:128], eng="scalar")
                if P2 < 5:
                    continue
                psv = PSB(1)
                P.mm(psv[:, 0:128], wT.all(), S[:, 384:512])
                P.mm(psv[:, 128:256], PT[:, C_GQT:C_GQT + 128], S[:, 384:512])
                vn = vnp.next()
                P.stt(vn.all(), psv[:, 0:128], -1.0, Xc[:, 0:128], ALU.mult, ALU.add)
                pso = PSB(2)
                P.mm(pso[:, 0:128], AT.all(), vn.all())
                if step < NT - 1:
                    kg = kgp.next()
                    P.ts(kg.all(), PT[:, C_GKTOK:C_GKTOK + 128], ek[:, 5:6])
                    P.mm(pso[:, 128:256], kg.all(), vn.all())
                P.copy(ysb[:, 384:512], pso[:, 0:128], eng="scalar")
                P.stt(yo[:, 384:512], psv[:, 128:256], e12[:, 5:6], ysb[:, 384:512], ALU.mult, ALU.add)
                if step < NT - 1:
                    P.ts(S2[:, 384:512], S[:, 384:512], e12[:, 11:12])
                    P.tt(S2[:, 384:512], pso[:, 128:256], S2[:, 384:512], ALU.add)
                P.dma("sync", YDIR[d][t].all(), yo.all())


def pass3(P, NT, PREP, YDIR, YOUT, rowb, Pool):
    with ExitStack() as s3:
        PTp = Pool("PT3", [128, PW], 2, F32, s3)
        yfp = Pool("yf", [128, 512], 2, F32, s3)
        ybp = Pool("yb", [128, 512], 2, F32, s3)
        sgp = Pool("sg", [128, 512], 2, F32, s3)
        op = Pool("o3", [128, 512], 2, F32, s3)
        tp = Pool("t3", [128, 256], 2, F32, s3)
        stp = Pool("st3", [128, 16], 3, F32, s3)
        for t in range(NT):
            PT = PTp.next()
            P.dma("sync", PT.all(), PREP[t].all())
            yf, yb = yfp.next(), ybp.next()
            P.dma("sync", yf.all(), YDIR[0][t].all())
            P.dma("sync", yb.all(), YDIR[1][t].all())
            P.tt(yf.all(), yf.all(), yb.all(), ALU.add)
            sg = sgp.next()
            P.act(sg[:, 0:256], PT[:, C_Z:C_Z + 256], AF.Silu)
            P.act(sg[:, 256:384], PT[:, C_RG:C_RG + 128], AF.Silu)
            P.act(sg[:, 384:512], PT[:, C_GG:C_GG + 128], AF.Silu)
            o = op.next()
            tmp = tp.next()
            P.tt(tmp.all().re("p (h q) -> p h q", q=64), PT[:, C_XS:C_XS + 256].re("p (h q) -> p h q", q=64),
                 rowb[:, 20:24].un(2).bc([128, 4, 64]), ALU.mult)
            P.tt(tmp.all(), tmp.all(), yf[:, 0:256], ALU.add)
            P.tt(o[:, 0:256], tmp.all(), sg[:, 0:256], ALU.mult)
            s = stp.next()
            tq = tp.next()
            P.rsum(s[:, 0:1], yf[:, 256:384])
            P.tt(tq[:, 0:128], yf[:, 256:384], yf[:, 256:384], ALU.mult)
            P.rsum(s[:, 1:2], tq[:, 0:128])
            P.ts(s[:, 2:3], s[:, 0:1], 1.0 / 128)
            P.tt(s[:, 3:4], s[:, 2:3], s[:, 2:3], ALU.mult)
            P.stt(s[:, 4:5], s[:, 1:2], 1.0 / 128, s[:, 3:4], ALU.mult, ALU.subtract)
            P.act(s[:, 5:6], s[:, 4:5], AF.Ln, bias=EPS); P.act(s[:, 5:6], s[:, 5:6], AF.Exp, scale=-0.5)
            P.ts(tq[:, 0:128], yf[:, 256:384], s[:, 2:3], s[:, 5:6], op0=ALU.subtract, op1=ALU.mult)
            P.tt(tq[:, 0:128], tq[:, 0:128], rowb[:, 26:154], ALU.mult)
            P.tt(o[:, 256:384], tq[:, 0:128], sg[:, 256:384], ALU.mult)
            P.tt(tq[:, 128:256], yf[:, 384:512], yf[:, 384:512], ALU.mult)
            P.rsum(s[:, 8:9], tq[:, 128:256])
            P.ts(s[:, 9:10], s[:, 8:9], 1.0 / 128, EPS, op0=ALU.mult, op1=ALU.add)
            P.act(s[:, 10:11], s[:, 9:10], AF.Ln); P.act(s[:, 10:11], s[:, 10:11], AF.Exp, scale=-0.5)
            P.ts(tq[:, 128:256], yf[:, 384:512], s[:, 10:11])
            P.tt(tq[:, 128:256], tq[:, 128:256], rowb[:, 154:282], ALU.mult)
            P.tt(o[:, 384:512], tq[:, 128:256], sg[:, 384:512], ALU.mult)
            P.dma("sync", YOUT[t], o.all())


from contextlib import ExitStack
import numpy as np

D = 2048
EPS = 1e-6
ALPHA = 8.0 ** 0.25
FG = 2


class Pool:
    def __init__(s, P, name, shape, n, dt=F32, stack=None):
        s.b = [P.sb(name + str(i), shape, dt, stack) for i in range(n)]
        s.i = 0

    def next(s):
        b = s.b[s.i % len(s.b)]
        s.i += 1
        return b


def ln_stats(P, stp, sqp, x):
    sq = sqp.next()
    P.act(sq.all(), x, AF.Square)
    s = stp.next()
    P.rsum(s[:, 0:1], x)
    P.rsum(s[:, 1:2], sq.all())
    P.ts(s[:, 2:3], s[:, 0:1], 1.0 / D)
    P.tt(s[:, 3:4], s[:, 2:3], s[:, 2:3], ALU.mult)
    P.stt(s[:, 4:5], s[:, 1:2], 1.0 / D, s[:, 3:4], ALU.mult, ALU.subtract)
    P.act(s[:, 5:6], s[:, 4:5], AF.Ln, bias=EPS)
    P.act(s[:, 5:6], s[:, 5:6], AF.Exp, scale=-0.5)
    P.stt(s[:, 6:7], s[:, 2:3], -1.0, s[:, 5:6], ALU.mult, ALU.mult)
    return s


def build_C(NTc, E, NF, has_ctx):
    moe = E > 1
    DFF = NF * 128
    nc = bass.Bass("TRN2", target_bir_lowering=False)
    dxs = nc.dram_tensor("xs", [NTc, 128, D], F32, kind="ExternalInput")
    dys = nc.dram_tensor("ys", [NTc, 128, D], F32, kind="ExternalInput")
    dwo = nc.dram_tensor("wout", [D, D], F32, kind="ExternalInput")
    dmc = nc.dram_tensor("mcols", [128, 5, 16], F32, kind="ExternalInput")
    drw = nc.dram_tensor("rows", [8, D], F32, kind="ExternalInput")
    dw1 = nc.dram_tensor("w1", [E, D, DFF], F32, kind="ExternalInput")
    dw3 = nc.dram_tensor("w3", [E, D, DFF], F32, kind="ExternalInput")
    dw2 = nc.dram_tensor("w2", [E, DFF, D], F32, kind="ExternalInput")
    did = nc.dram_tensor("ident", [128, 128], F32, kind="ExternalInput")
    if moe:
        drt = nc.dram_tensor("router", [D, 8], F32, kind="ExternalInput")
    dxo = nc.dram_tensor("xo", [NTc, 128, D], F32, kind="ExternalOutput")
    with ExitStack() as st:
        P = Prog(nc, st)
        XS, YS, WO, MC, RW = Buf(dxs, "xs"), Buf(dys, "ys"), Buf(dwo, "wout"), Buf(dmc, "mcols"), Buf(drw, "rows")
        W1, W3, W2, IDN, XO = Buf(dw1, "w1"), Buf(dw3, "w3"), Buf(dw2, "w2"), Buf(did, "ident"), Buf(dxo, "xo")
        X1 = [P.dram("x1_%d" % t, [128, D]) for t in range(NTc)]
        HT = [P.dram("ht_%d" % t, [128, 16, 128], BF16) for t in range(NTc)]
        GT = [P.dram("gt_%d" % t, [128, 8]) for t in range(NTc)]
        FF = [P.dram("ff_%d" % t, [128, D]) for t in range(NTc)]
        ident = P.sb("ident", [128, 128])
        P.dma("sync", ident.all(), IDN.all())
        IDENT = ident.all()
        mc = P.sb("mc", [128, 5, 16])
        P.dma("sync", mc.all(), MC.all())
        P.ts(mc[:, 0, :], mc[:, 0, :], 1.0, op0=ALU.add)
        P.ts(mc[:, 2, :], mc[:, 2, :], 1.0, op0=ALU.add)
        scr = P.sb("scr", [128, 8])
        ps_pool = [P.ps("ps%d" % i, [128, 512]) for i in range(8)]
        psi = [0]

        def PS4():
            b = ps_pool[psi[0] % 4]
            psi[0] += 1
            return b

        def is_ctx(t):
            return has_ctx and t == NTc - 1

        def bcast_row(r):
            return View(RW, RW.t[r:r + 1, :].broadcast_to([128, D]))

        blocks = [list(range(b, min(b + 4, NTc))) for b in range(0, NTc, 4)]
        with ExitStack() as s1:
            rowt = {}
            for nm, r in (("m2l", 0), ("m2c", 1), ("g1", 2), ("b1", 3)):
                if nm == "m2c" and not has_ctx:
                    continue
                rowt[nm] = P.sb("row_" + nm, [128, D], F32, s1)
                P.dma("sync", rowt[nm].all(), bcast_row(r))
            if moe:
                rt = P.sb("rt", [128, 16, 8], F32, s1)
                P.dma("sync", rt.all(), Buf(drt, "router").all().re("(k p) e -> p k e", p=128))
            ytp = Pool(P, "yt", [128, D], 1, F32, s1)
            sqp = Pool(P, "sq", [128, D], 1, BF16, s1)
            stp = Pool(P, "st", [128, 16], 4, F32, s1)
            yTp = Pool(P, "yT", [128, 16, 128], 4, BF16, s1)
            wop = Pool(P, "wo", [128, 16, 512], 2, BF16, s1)
            ob = P.sb("o", [128, 4, D], F32, s1)
            xtp = Pool(P, "xt", [128, D], 1, F32, s1)
            tmpp = Pool(P, "tmp", [128, D], 2, F32, s1)
            hTp = Pool(P, "hTt", [128, 16, 128], 2, BF16, s1)
            if moe:
                hfp = Pool(P, "hTf", [128, 16, 128], 1, F32, s1)
                gp = Pool(P, "gt", [128, 8], 8, F32, s1)
            for blk in blocks:
                yTs = []
                for t in blk:
                    yt = ytp.next()
                    P.dma("sync", yt.all(), YS[t])
                    sq = sqp.next()
                    P.act(sq[:, 0:1024], yt[:, 0:1024], AF.Square)
                    s = stp.next()
                    P.rsum(s[:, 0:1], sq[:, 0:1024])
                    P.act(s[:, 1:2], s[:, 0:1], AF.Ln, bias=EPS, scale=1.0 / 1024)
                    P.act(s[:, 1:2], s[:, 1:2], AF.Exp, scale=-0.5)
                    P.ts(yt[:, 0:1024], yt[:, 0:1024], s[:, 1:2])
                    yT = yTp.next()
                    yTs.append(yT)
                    for kb in range(4):
                        ps = PS4()
                        for j in range(4):
                            k = kb * 4 + j
                            P.tr(ps[:, j * 128:(j + 1) * 128], yt[:, k * 128:(k + 1) * 128], IDENT)
                        for j in range(4):
                            k = kb * 4 + j
                            P.act(yT[:, k, :], ps[:, j * 128:(j + 1) * 128], AF.Identity, scale=mc[:, 4, k:k + 1])
                for cg in range(4):
                    wo = wop.next()
                    P.dma("gpsimd", wo.all(), WO[:, cg * 512:(cg + 1) * 512].re("(k p) n -> p k n", p=128))
                    for j, t in enumerate(blk):
                        ps = PS4()
                        for k in range(16):
                            P.mm(ps[:, 0:512], yTs[j][:, k, :], wo[:, k, :], start=(k == 0), stop=(k == 15))
                        P.copy(ob[:, j, cg * 512:(cg + 1) * 512], ps[:, 0:512], eng="scalar")
                for j, t in enumerate(blk):
                    cx = is_ctx(t)
                    xt = xtp.next()
                    P.dma("sync", xt.all(), XS[t])
                    tmp = tmpp.next()
                    P.tt(tmp.all(), ob[:, j, :], rowt["m2c" if cx else "m2l"].all(), ALU.mult)
                    P.stt(tmp.all(), xt.all(), ALPHA, tmp.all(), ALU.mult, ALU.add)
                    s = ln_stats(P, stp, sqp, tmp.all())
                    xh = tmpp.next()
                    P.act(xh.all(), tmp.all(), AF.Identity, bias=s[:, 6:7], scale=s[:, 5:6])
                    P.tt(xh.all(), xh.all(), rowt["g1"].all(), ALU.mult)
                    P.tt(ob[:, j, :], xh.all(), rowt["b1"].all(), ALU.add)
                    P.dma("sync", X1[t].all(), ob[:, j, :])
                    s2 = ln_stats(P, stp, sqp, ob[:, j, :])
                    xh2 = tmpp.next()
                    P.act(xh2.all(), ob[:, j, :], AF.Identity, bias=s2[:, 6:7], scale=s2[:, 5:6])
                    hTt = hTp.next()
                    mi = 2 if cx else 0
                    if moe:
                        hf = hfp.next()
                    for kb in range(4):
                        ps = PS4()
                        for jj in range(4):
                            k = kb * 4 + jj
                            P.tr(ps[:, jj * 128:(jj + 1) * 128], xh2[:, k * 128:(k + 1) * 128], IDENT)
                        for jj in range(4):
                            k = kb * 4 + jj
                            P.act(hTt[:, k, :], ps[:, jj * 128:(jj + 1) * 128], AF.Identity, bias=mc[:, mi + 1, k:k + 1], scale=mc[:, mi, k:k + 1])
                            if moe:
                                P.act(hf[:, k, :], ps[:, jj * 128:(jj + 1) * 128], AF.Identity, bias=mc[:, mi + 1, k:k + 1], scale=mc[:, mi, k:k + 1])
                    P.dma("sync", HT[t].all(), hTt.all())
                    if moe:
                        ps = PS4()
                        for k in range(16):
                            P.mm(ps[:, 0:8], hf[:, k, :], rt[:, k, :], start=(k == 0), stop=(k == 15))
                        lg, w = gp.next(), gp.next()
                        P.copy(lg.all(), ps[:, 0:8], eng="scalar")
                        sm = stp.next()
                        P.rmax(sm[:, 0:1], lg.all())
                        P.ts(w.all(), lg.all(), sm[:, 0:1], op0=ALU.is_equal)
                        P.stt(w.all(), w.all(), -1e30, lg.all(), ALU.mult, ALU.add)
                        P.rmax(sm[:, 1:2], w.all())
                        P.ts(w.all(), lg.all(), sm[:, 1:2], op0=ALU.is_ge)
                        P.ts(sm[:, 2:3], sm[:, 0:1], -1.0)
                        ex = gp.next()
                        P.act(ex.all(), lg.all(), AF.Exp, bias=sm[:, 2:3])
                        P.tt(ex.all(), ex.all(), w.all(), ALU.mult)
                        P.rsum(sm[:, 3:4], ex.all())
                        P.recip(sm[:, 4:5], sm[:, 3:4])
                        gt = gp.next()
                        P.ts(gt.all(), ex.all(), sm[:, 4:5])
                        P.dma("sync", GT[t].all(), gt.all())
        P.barrier(scr[:, 0:1])
        with ExitStack() as s2:
            hTb = P.sb("hTb", [128, 16, 512], BF16, s2)
            uT = P.sb("uT", [128, NF, 512], BF16, s2)
            w1p = Pool(P, "w1g", [128, 16, FG * 128], 2, BF16, s2)
            w3p = Pool(P, "w3g", [128, 16, FG * 128], 2, BF16, s2)
            NG2 = 11 if NF % 11 == 0 else NF
            w2p = Pool(P, "w2g", [128, NG2, 512], 2, BF16, s2)
            acc = P.sb("acc", [128, 4, D], F32, s2)
            stm = Pool(P, "stmp", [128, 512], 2, F32, s2)
            dtm = Pool(P, "dtmp", [128, 512], 2, F32, s2)
            gtb = P.sb("gtb", [128, 4, 8], F32, s2)
            for blk in blocks:
                nt = len(blk)
                NTOK = nt * 128
                for j, t in enumerate(blk):
                    P.dma("sync", hTb[:, :, j * 128:(j + 1) * 128], HT[t].all())
                    if moe:
                        P.dma("sync", gtb[:, j, :], GT[t].all())
                for e in range(E):
                    for fg in range(NF // FG):
                        w1g, w3g = w1p.next(), w3p.next()
                        P.dma("gpsimd", w1g.all(), W1[e][:, fg * FG * 128:(fg + 1) * FG * 128].re("(k p) n -> p k n", p=128))
                        P.dma("gpsimd", w3g.all(), W3[e][:, fg * FG * 128:(fg + 1) * FG * 128].re("(k p) n -> p k n", p=128))
                        for jf in range(FG):
                            fc = fg * FG + jf
                            psa, psb = PS4(), PS4()
                            for k in range(16):
                                P.mm(psa[:, 0:NTOK], w1g[:, k, jf * 128:(jf + 1) * 128], hTb[:, k, 0:NTOK], start=(k == 0), stop=(k == 15))
                            for k in range(16):
                                P.mm(psb[:, 0:NTOK], w3g[:, k, jf * 128:(jf + 1) * 128], hTb[:, k, 0:NTOK], start=(k == 0), stop=(k == 15))
                            sm_ = stm.next()
                            P.act(sm_[:, 0:NTOK], psa[:, 0:NTOK], AF.Silu)
                            P.stt(uT[:, fc, 0:NTOK], psb[:, 0:NTOK], 1.0, sm_[:, 0:NTOK], ALU.mult, ALU.mult)
                    for cg in range(4):
                        pds = [ps_pool[4 + j] for j in range(nt)]
                        for g2 in range(NF // NG2):
                            w2g = w2p.next()
                            P.dma("gpsimd", w2g.all(), W2[e][g2 * NG2 * 128:(g2 + 1) * NG2 * 128, cg * 512:(cg + 1) * 512].re("(c p) n -> p c n", p=128))
                            for c in range(NG2):
                                fc = g2 * NG2 + c
                                for j in range(nt):
                                    P.mm(pds[j][:, 0:512], uT[:, fc, j * 128:(j + 1) * 128], w2g[:, c, :], start=(fc == 0), stop=(fc == NF - 1))
                        for j in range(nt):
                            dst = acc[:, j, cg * 512:(cg + 1) * 512]
                            if not moe:
                                P.copy(dst, pds[j][:, 0:512], eng="scalar")
                            elif e == 0:
                                P.act(dst, pds[j][:, 0:512], AF.Identity, scale=gtb[:, j, e:e + 1])
                            else:
                                dt_ = dtm.next()
                                P.act(dt_.all(), pds[j][:, 0:512], AF.Identity, scale=gtb[:, j, e:e + 1])
                                P.tt(dst, dst, dt_.all(), ALU.add)
                for j, t in enumerate(blk):
                    P.dma("sync", FF[t].all(), acc[:, j, :])
        P.barrier(scr[:, 0:1])
        with ExitStack() as s3:
            rowt = {}
            for nm, r in (("m5l", 4), ("m5c", 5), ("g2", 6), ("b2", 7)):
                if nm == "m5c" and not has_ctx:
                    continue
                rowt[nm] = P.sb("row_" + nm, [128, D], F32, s3)
                P.dma("sync", rowt[nm].all(), bcast_row(r))
            x1p = Pool(P, "x1t", [128, D], 2, F32, s3)
            fp = Pool(P, "ft", [128, D], 2, F32, s3)
            tp = Pool(P, "t3", [128, D], 2, F32, s3)
            sqp = Pool(P, "sq3", [128, D], 1, BF16, s3)
            stp = Pool(P, "st3", [128, 16], 4, F32, s3)
            for t in range(NTc):
                cx = is_ctx(t)
                x1t, ft = x1p.next(), fp.next()
                P.dma("sync", x1t.all(), X1[t].all())
                P.dma("sync", ft.all(), FF[t].all())
                P.tt(ft.all(), ft.all(), rowt["m5c" if cx else "m5l"].all(), ALU.mult)
                P.stt(ft.all(), x1t.all(), ALPHA, ft.all(), ALU.mult, ALU.add)
                s = ln_stats(P, stp, sqp, ft.all())
                xh = tp.next()
                P.act(xh.all(), ft.all(), AF.Identity, bias=s[:, 6:7], scale=s[:, 5:6])
                P.tt(xh.all(), xh.all(), rowt["g2"].all(), ALU.mult)
                P.tt(xh.all(), xh.all(), rowt["b2"].all(), ALU.add)
                P.dma("sync", XO[t], xh.all())
        P.barrier(scr[:, 0:1])
        P.emit()
    return nc


def build_M(NCOLS):
    nc = bass.Bass("TRN2", target_bir_lowering=False)
    dct = nc.dram_tensor("cT", [128, 16, 3], F32, kind="ExternalInput")
    dwm = nc.dram_tensor("wm", [D, NCOLS], F32, kind="ExternalInput")
    dbm = nc.dram_tensor("bm", [1, NCOLS], F32, kind="ExternalInput")
    dmo = nc.dram_tensor("mo", [3, NCOLS], F32, kind="ExternalOutput")
    with ExitStack() as st:
        P = Prog(nc, st)
        CT, WM, BM, MO = Buf(dct, "cT"), Buf(dwm, "wm"), Buf(dbm, "bm"), Buf(dmo, "mo")
        ca = P.sb("ca", [128, 16, 3])
        P.dma("sync", ca.all(), CT.all())
        P.act(ca.all(), ca.all(), AF.Silu)
        bmt = P.sb("bmt", [3, NCOLS])
        P.dma("sync", bmt.all(), View(BM, BM.t[0:1, :].broadcast_to([3, NCOLS])))
        scr = P.sb("scr", [128, 8])
        ps_pool = [P.ps("ps%d" % i, [128, 512]) for i in range(4)]
        wp = Pool(P, "w", [128, 16, 512], 2)
        op = Pool(P, "o", [3, 512], 2)
        for g in range(NCOLS // 512):
            w = wp.next()
            P.dma("sync", w.all(), WM[:, g * 512:(g + 1) * 512].re("(k p) n -> p k n", p=128))
            ps = ps_pool[g % 4]
            for k in range(16):
                P.mm(ps[0:3, 0:512], ca[:, k, :], w[:, k, :], start=(k == 0), stop=(k == 15))
            o = op.next()
            P.copy(o.all(), ps[0:3, 0:512], eng="scalar")
            P.tt(o.all(), o.all(), bmt[:, g * 512:(g + 1) * 512], ALU.add)
            P.dma("sync", MO[:, g * 512:(g + 1) * 512], o.all())
        P.barrier(scr[:, 0:1])
        P.emit()
    return nc


import numpy as np

D = 2048
BIG = 30000.0
SSD_COLS = 1024 + 1536 + 32
RET_COLS = 2048
RB = SSD_COLS
GB = SSD_COLS + RET_COLS


def col_index(g):
    gr = g // 2
    r = np.arange(128)
    fm = [1024 + 256 * g + r, 1024 + 256 * g + 128 + r, 2048 + gr * 128 + r, 2048 + 256 + gr * 128 + r,
          GB + g * 128 + r, GB + 512 + g * 128 + r, GB + 1024 + g * 128 + r]
    tma = [256 * g + np.arange(256)]
    tma += [2560 + d * 16 + 4 * g + np.arange(4) for d in range(2)]
    tma += [np.array([GB + 2048 + d * 4 + g for d in range(2)])]
    tma += [np.array([GB + 2048 + 8 + d * 4 + g for d in range(2)])]
    tma += [GB + 1536 + g * 128 + r]
    tmb = [RB + j * 512 + g * 128 + r for j in range(4)]
    return np.concatenate(fm + tma + tmb)


def consts():
    j = np.arange(128)[:, None]
    l = np.arange(128)[None, :]
    c = np.zeros((8, 128, 128), np.float32)
    c[0] = np.eye(128)
    c[1] = 1.0
    c[2] = (j <= l)
    c[3] = (j >= l)
    c[4] = np.where(l >= j, 0.0, -BIG)
    c[5] = np.where(l <= j, 0.0, -BIG)
    c[6] = np.where(l < j, 0.0, BIG)
    c[7] = np.where(l > j, 0.0, BIG)
    return c


def rope_tiles(L):
    n_freq = 32
    inv = (10000.0 ** (-np.arange(n_freq, dtype=np.float32) / n_freq)).astype(np.float32)
    pos = np.arange(L)
    row = (pos // 64).astype(np.float32)
    col = (pos % 64).astype(np.float32)
    ang = np.concatenate([row[:, None] * inv, col[:, None] * inv], axis=-1).astype(np.float32)
    cos, sin = np.cos(ang).astype(np.float32), np.sin(ang).astype(np.float32)
    s = np.float32(128.0 ** -0.5)
    lat = np.concatenate([cos, sin, cos * s, sin * s], axis=-1).reshape(L // 128, 128, 256)
    ctx = np.zeros((2, 128, 256), np.float32)
    ctx[:, :, 0:64] = 1.0
    ctx[:, :, 128:192] = s
    return np.concatenate([ctx, lat], axis=0).astype(np.float32)


def colvec(v):
    return np.ascontiguousarray(v.reshape(16, 128).T)


def prep_B(inp, i, x_l, x_c, mod_l, mod_c, L):
    maps = []
    cst = consts()
    rope = rope_tiles(L)
    for core in range(8):
        b, g = core // 4, core % 4
        gr = g // 2
        x = np.concatenate([x_c[b], x_l[b]], axis=0).reshape(-1, 128, D)
        ml = mod_l[b].reshape(6, D)
        mc = mod_c.reshape(6, D)
        modc = np.stack([colvec(ml[1]), colvec(ml[0]), colvec(mc[1]), colvec(mc[0])], axis=1)
        win = np.ascontiguousarray(inp['w_in'][i][:, col_index(g)])
        r = np.arange(128)
        sch = [256 * g + r, 256 * g + 128 + r, 1024 + gr * 128 + r, 1024 + 256 + gr * 128 + r]
        gch = [g * 128 + r, 512 + g * 128 + r, 1024 + g * 128 + r]
        cw = np.zeros((128, 7, 5), np.float32)
        cb = np.zeros((128, 7), np.float32)
        for bi, ch in enumerate(sch):
            cw[:, bi, :] = inp['ssd_conv_w'][i][:, ch].T
            cb[:, bi] = inp['ssd_conv_b'][i][ch]
        for bi, ch in enumerate(gch):
            cw[:, 4 + bi, :] = inp['gdn_conv_w'][i][:, ch].T
        hs = slice(4 * g, 4 * g + 4)
        rowp = np.concatenate([
            inp['ssd_dt_bias'][i][0, hs], inp['ssd_dt_bias'][i][1, hs], inp['gdn_dt_bias'][i][:, g],
            inp['ssd_a_log'][i][0, hs], inp['ssd_a_log'][i][1, hs], inp['gdn_a_log'][i][:, g],
            inp['ssd_d'][i][hs], inp['ret_log_decay'][i][:, g],
            inp['ret_norm_w'][i][g * 128:(g + 1) * 128], inp['gdn_norm_w'][i]]).astype(np.float32)[None, :]
        maps.append({"x": np.ascontiguousarray(x, np.float32), "modc": np.ascontiguousarray(modc, np.float32), "win": win,
                     "cw": cw, "cb": cb, "rowp": rowp, "consts": cst, "rope": rope})
    return maps


from concourse.bass_utils import run_bass_kernel_spmd

_PROG = {}


def _prog(key, builder):
    if key not in _PROG:
        _PROG[key] = builder()
    return _PROG[key]


def _run(nc, maps):
    return run_bass_kernel_spmd(nc, maps, core_ids=list(range(8))).results


def kernel(**inputs):
    inp = {k: np.asarray(v) for k, v in inputs.items()}
    f32 = np.float32
    L = inp['x'].shape[1]
    NTL = L // 128
    NT = 2 + NTL
    nl = NTL // 4
    NF = inp['ffn_w1'].shape[2] // 128
    cvec = np.stack([inp['c'][0], inp['c'][1], inp['c_ctx']]).astype(f32)
    cT = np.ascontiguousarray(cvec.reshape(3, 16, 128).transpose(2, 1, 0))
    maps = []
    for core in range(8):
        cols = slice(core * 1536, (core + 1) * 1536)
        wm = np.ascontiguousarray(np.concatenate([inp['w_mod'][i][:, cols] for i in range(4)], axis=1))
        bm = np.ascontiguousarray(np.concatenate([inp['b_mod'][i][cols] for i in range(4)])[None, :])
        maps.append({"cT": cT, "wm": wm, "bm": bm})
    res = _run(_prog("M", lambda: build_M(6144)), maps)
    mod = np.zeros((4, 3, 12288), f32)
    for core in range(8):
        mo = res[core]["mo"]
        for i in range(4):
            mod[i][:, core * 1536:(core + 1) * 1536] = mo[:, i * 1536:(i + 1) * 1536]
    x_l = np.array(inp['x'], dtype=f32, copy=True)
    x_c = np.array(inp['ctx'], dtype=f32, copy=True)
    ident = np.eye(128, dtype=f32)
    ones1024 = np.ones(1024, f32)
    for i in range(4):
        last = i == 3
        moe = i % 2 == 1
        j = i // 2
        mapsB = prep_B(inp, i, x_l, x_c, mod[i][0:2], mod[i][2], L)
        resB = _run(_prog(("B", NT), lambda: build_B(NT, 3)), mapsB)
        yall = np.zeros((2, NT * 128, 2048), f32)
        for core in range(8):
            b, g = divmod(core, 4)
            yo = resB[core]["yout"].reshape(NT * 128, 512)
            yall[b][:, 256 * g:256 * g + 256] = yo[:, 0:256]
            yall[b][:, 1024 + 128 * g:1024 + 128 * g + 128] = yo[:, 256:384]
            yall[b][:, 1536 + 128 * g:1536 + 128 * g + 128] = yo[:, 384:512]
        NTc = nl + (0 if last else 1)
        if moe:
            w1, w3, w2 = inp['moe_w1'][j], inp['moe_w3'][j], inp['moe_w2'][j]
            router = np.ascontiguousarray(inp['moe_router'][j])
        else:
            w1, w3, w2 = inp['ffn_w1'][j][None], inp['ffn_w3'][j][None], inp['ffn_w2'][j][None]
        w1, w3, w2 = np.ascontiguousarray(w1), np.ascontiguousarray(w3), np.ascontiguousarray(w2)
        wout = np.ascontiguousarray(inp['w_out'][i])
        mc_ = mod[i][2].reshape(6, 2048)
        snw = colvec(np.concatenate([inp['ssd_norm_w'][i], ones1024]).astype(f32))
        mapsC = []
        for core in range(8):
            b, q = divmod(core, 4)
            xs = np.zeros((NTc, 128, 2048), f32)
            ys = np.zeros((NTc, 128, 2048), f32)
            xs[:nl] = x_l[b][q * nl * 128:(q + 1) * nl * 128].reshape(nl, 128, 2048)
            ys[:nl] = yall[b][256 + q * nl * 128:256 + (q + 1) * nl * 128].reshape(nl, 128, 2048)
            if not last:
                xs[nl][:64] = x_c[b][q * 64:(q + 1) * 64]
                ys[nl][:64] = yall[b][q * 64:(q + 1) * 64]
            ml = mod[i][b].reshape(6, 2048)
            mcols = np.ascontiguousarray(np.stack([colvec(ml[4]), colvec(ml[3]), colvec(mc_[4]), colvec(mc_[3]), snw], axis=1), dtype=f32)
            rows = np.ascontiguousarray(np.stack([ml[2], mc_[2], inp['ln1_g'][i], inp['ln1_b'][i], ml[5], mc_[5], inp['ln2_g'][i], inp['ln2_b'][i]]), dtype=f32)
            m = {"xs": xs, "ys": ys, "wout": wout, "mcols": mcols, "rows": rows, "w1": w1, "w3": w3, "w2": w2, "ident": ident}
            if moe:
                m["router"] = router
            mapsC.append(m)
        E = 8 if moe else 1
        has_ctx = not last
        resC = _run(_prog(("C", NTc, E, NF, has_ctx), lambda: build_C(NTc, E, NF, has_ctx)), mapsC)
        for core in range(8):
            b, q = divmod(core, 4)
            xo = resC[core]["xo"]
            x_l[b][q * nl * 128:(q + 1) * nl * 128] = xo[:nl].reshape(nl * 128, 2048)
            if not last:
                x_c[b][q * 64:(q + 1) * 64] = xo[nl][:64]
    return x_l.astype(np.float32)
```
